# Optimizing a Trainium2 kernel written in Bass

```python
import jax
import jax.numpy as jnp
from jax import lax
import numpy as np

D_MODEL = 1024
BATCH = 16
SEQ = 256
DEPTH = 4
DEC_BATCH = 2
DEC_SEQ = 4096
PAST_LEN = 256

GRID_W = 64
N_EVEN = (DEPTH + 1) // 2
N_ODD = DEPTH // 2
C_A = D_MODEL // 2
G_A = 8
CHUNK = 128
C_B = D_MODEL // 2
CONV_K = 31
N_HEADS_C = 16
HEAD_DIM = D_MODEL // N_HEADS_C
WIN_ROWS = 8
WIN_COLS = 16
N_EXPERTS = 32
TOP_K = 4
D_EXPERT = D_MODEL
SWIGLU_ALPHA = 1.702
SWIGLU_LIMIT = 7.0
MOE_BLOCK = 128
DEEPNORM_ALPHA = (2 * DEPTH) ** 0.25
DEEPNORM_BETA = (8 * DEPTH) ** -0.25
LN_EPS = 1e-5
NEG_INF = -1e30

kernel_name = 'hybrid_gmlp_conv_natten_moe_diffusion_step'


def layer_norm(x, g, b):
    xf = x.astype(jnp.float32)
    mu = jnp.mean(xf, axis=-1, keepdims=True)
    var = jnp.mean(jnp.square(xf - mu), axis=-1, keepdims=True)
    y = (xf - mu) * lax.rsqrt(var + LN_EPS) * g.astype(jnp.float32) + b.astype(jnp.float32)
    return y.astype(x.dtype)


def adaln(cvec, w_mod_l, b_mod_l):
    m = jax.nn.silu(cvec) @ w_mod_l + b_mod_l
    return m.reshape(cvec.shape[0], 6, 1, D_MODEL)


def mixer_ab(h, w_in, sgu_g, sgu_b, w_sp, b_sp, conv_w, conv_b, cln_g, cln_b, w_out):
    bn, n, _ = h.shape
    z = h @ w_in
    u, v, a, g = jnp.split(z, [C_A, 2 * C_A, 2 * C_A + C_B], axis=-1)
    u = jax.nn.gelu(u)
    v = layer_norm(jax.nn.gelu(v), sgu_g, sgu_b)
    vc = v.reshape(bn, n // CHUNK, CHUNK, G_A, C_A // G_A)
    s = jnp.einsum('gpq,bnqgc->bnpgc', w_sp, vc) + b_sp.T[None, None, :, :, None]
    y_a = u * s.reshape(bn, n, C_A)
    gl = a * jax.nn.sigmoid(g)
    dc = lax.conv_general_dilated(
        gl, conv_w[:, None, :].astype(gl.dtype), window_strides=(1,),
        padding=[(CONV_K // 2, CONV_K // 2)], dimension_numbers=('NWC', 'WIO', 'NWC'),
        feature_group_count=C_B) + conv_b
    y_b = jax.nn.silu(layer_norm(dc, cln_g, cln_b))
    return jnp.concatenate([y_a, y_b], axis=-1) @ w_out


def attn_ctx(h, w_qkv, w_out):
    bn, n, _ = h.shape
    q, k, v = jnp.split(h @ w_qkv, 3, axis=-1)
    q = q.reshape(bn, n, N_HEADS_C, HEAD_DIM)
    k = k.reshape(bn, n, N_HEADS_C, HEAD_DIM)
    v = v.reshape(bn, n, N_HEADS_C, HEAD_DIM)
    s = jnp.einsum('bqhd,bkhd->bhqk', q, k).astype(jnp.float32) * (HEAD_DIM ** -0.5)
    p = jax.nn.softmax(s, axis=-1).astype(v.dtype)
    o = jnp.einsum('bhqk,bkhd->bqhd', p, v).reshape(bn, n, D_MODEL)
    return o @ w_out, k, v


def attn_latent(h, ck, cv, w_qkv, rpb, w_out):
    bn, n, _ = h.shape
    rows_n = n // GRID_W
    wr = min(WIN_ROWS, rows_n)
    m_blocks = GRID_W // WIN_COLS
    kbw = 2 * WIN_COLS
    q, k, v = jnp.split(h @ w_qkv, 3, axis=-1)
    q = q.reshape(bn, rows_n, m_blocks, WIN_COLS, N_HEADS_C, HEAD_DIM)
    k = k.reshape(bn, rows_n, GRID_W, N_HEADS_C, HEAD_DIM)
    v = v.reshape(bn, rows_n, GRID_W, N_HEADS_C, HEAD_DIM)
    r = jnp.arange(rows_n)
    rows = jnp.clip(r - wr // 2, 0, rows_n - wr)[:, None] + jnp.arange(wr)
    mb = jnp.arange(m_blocks)
    cols = jnp.clip(mb * WIN_COLS - WIN_COLS // 2, 0, GRID_W - kbw)[:, None] + jnp.arange(kbw)
    ridx = rows[:, None, :, None]
    cidx = cols[None, :, None, :]
    kb = k[:, ridx, cidx].reshape(bn, rows_n, m_blocks, wr * kbw, N_HEADS_C, HEAD_DIM)
    vb = v[:, ridx, cidx].reshape(bn, rows_n, m_blocks, wr * kbw, N_HEADS_C, HEAD_DIM)
    qcol = mb[:, None] * WIN_COLS + jnp.arange(WIN_COLS)
    qcs = jnp.clip(qcol - WIN_COLS // 2, 0, GRID_W - WIN_COLS)
    kc = cols[:, None, :]
    valid = (kc >= qcs[..., None]) & (kc < qcs[..., None] + WIN_COLS)
    valid = jnp.broadcast_to(valid[:, :, None, :], (m_blocks, WIN_COLS, wr, kbw))
    valid = valid.reshape(m_blocks, 1, WIN_COLS, wr * kbw)
    dc_idx = jnp.clip(kc - qcol[..., None] + WIN_COLS - 1, 0, 2 * WIN_COLS - 2)
    dr_idx = rows - r[:, None] + WIN_ROWS - 1
    bias = rpb[:, dr_idx[:, None, None, :, None], dc_idx[None, :, :, None, :]]
    bias = jnp.transpose(bias, (1, 2, 0, 3, 4, 5)).reshape(
        rows_n, m_blocks, N_HEADS_C, WIN_COLS, wr * kbw).astype(jnp.float32)
    scale = HEAD_DIM ** -0.5
    s_loc = jnp.einsum('brmqhd,brmkhd->brmhqk', q, kb).astype(jnp.float32) * scale + bias
    s_loc = jnp.where(valid, s_loc, NEG_INF)
    s_ctx = jnp.einsum('brmqhd,bkhd->brmhqk', q, ck).astype(jnp.float32) * scale
    p = jax.nn.softmax(jnp.concatenate([s_loc, s_ctx], axis=-1), axis=-1).astype(v.dtype)
    n_loc = wr * kbw
    o = (jnp.einsum('brmhqk,brmkhd->brmqhd', p[..., :n_loc], vb)
         + jnp.einsum('brmhqk,bkhd->brmqhd', p[..., n_loc:], cv))
    return o.reshape(bn, n, D_MODEL) @ w_out


def swiglu_clamped(hgu):
    x_glu, x_lin = jnp.split(hgu, 2, axis=-1)
    x_glu = jnp.minimum(x_glu, SWIGLU_LIMIT)
    x_lin = jnp.clip(x_lin, -SWIGLU_LIMIT, SWIGLU_LIMIT)
    return x_glu * jax.nn.sigmoid(SWIGLU_ALPHA * x_glu) * (x_lin + 1.0)


def moe(x, w_r, b_r, w_gu, b_gu, w_d, b_d):
    bn, n, d = x.shape
    t = bn * n
    xt = x.reshape(t, d)
    logits = (xt @ w_r + b_r).astype(jnp.float32)
    top_v, top_e = lax.top_k(logits, TOP_K)
    gates = jax.nn.softmax(top_v, axis=-1)
    n_assign = t * TOP_K
    e_flat = top_e.reshape(n_assign)
    tok_flat = jnp.repeat(jnp.arange(t, dtype=jnp.int32), TOP_K)
    g_flat = gates.reshape(n_assign)
    order = jnp.argsort(e_flat)
    e_s = e_flat[order]
    tok_s = tok_flat[order]
    g_s = g_flat[order]
    counts = jnp.bincount(e_flat, length=N_EXPERTS)
    padded = (counts + MOE_BLOCK - 1) // MOE_BLOCK * MOE_BLOCK
    pad_end = jnp.cumsum(padded)
    pad_start = pad_end - padded
    grp_start = jnp.cumsum(counts) - counts
    dest = pad_start[e_s] + jnp.arange(n_assign) - grp_start[e_s]
    n_blocks = -(-n_assign // MOE_BLOCK) + N_EXPERTS
    n_rows = n_blocks * MOE_BLOCK
    rows_tok = jnp.full((n_rows,), t, jnp.int32).at[dest].set(tok_s)
    rows_gate = jnp.zeros((n_rows,), jnp.float32).at[dest].set(g_s)
    block_e = jnp.minimum(
        jnp.searchsorted(pad_end, jnp.arange(n_blocks) * MOE_BLOCK, side='right'),
        N_EXPERTS - 1).astype(jnp.int32)
    x_pad = jnp.concatenate([xt, jnp.zeros((1, d), xt.dtype)], axis=0)
    xb = x_pad[rows_tok].reshape(n_blocks, MOE_BLOCK, d)

    def expert_block(args):
        xblk, e = args
        hgu = xblk @ w_gu[e] + b_gu[e]
        return swiglu_clamped(hgu) @ w_d[e] + b_d[e]

    yb = lax.map(expert_block, (xb, block_e)).reshape(n_rows, d)
    y = jnp.zeros((t + 1, d), x.dtype).at[rows_tok].add(yb * rows_gate[:, None].astype(yb.dtype))
    return y[:t].reshape(bn, n, d)


def setup_inputs(seed: int = 0) -> dict:
    key = jax.random.key(seed)
    ks = jax.random.split(key, 29)
    d = D_MODEL

    def nrm(k, shape, scale):
        return jax.random.normal(k, shape, jnp.float32) * scale

    return {
        'x_prompt': nrm(ks[0], (BATCH, SEQ, d), 1.0),
        'x_sample': nrm(ks[1], (DEC_BATCH, DEC_SEQ, d), 1.0),
        'c': nrm(ks[2], (DEC_BATCH, d), 1.0),
        'cache_k': nrm(ks[3], (DEC_BATCH, N_ODD, PAST_LEN, N_HEADS_C, HEAD_DIM), 1.0),
        'cache_v': nrm(ks[4], (DEC_BATCH, N_ODD, PAST_LEN, N_HEADS_C, HEAD_DIM), 1.0),
        'c_ctx': nrm(ks[5], (d,), 1.0),
        'w_mod': nrm(ks[6], (DEPTH, d, 6 * d), 0.5 * d ** -0.5),
        'b_mod': nrm(ks[7], (DEPTH, 6 * d), 0.02),
        'ln_g': 1.0 + nrm(ks[8], (DEPTH, 2, d), 0.02),
        'ln_b': nrm(ks[9], (DEPTH, 2, d), 0.02),
        'w_in_ab': nrm(ks[10], (N_EVEN, d, 2 * C_A + 2 * C_B), d ** -0.5),
        'sgu_ln_g': 1.0 + nrm(ks[11], (N_EVEN, C_A), 0.02),
        'sgu_ln_b': nrm(ks[12], (N_EVEN, C_A), 0.02),
        'w_spatial': nrm(ks[13], (N_EVEN, G_A, CHUNK, CHUNK), CHUNK ** -0.5),
        'b_spatial': 1.0 + nrm(ks[14], (N_EVEN, G_A, CHUNK), 0.02),
        'conv_w': nrm(ks[15], (N_EVEN, CONV_K, C_B), CONV_K ** -0.5),
        'conv_b': nrm(ks[16], (N_EVEN, C_B), 0.02),
        'conv_ln_g': 1.0 + nrm(ks[17], (N_EVEN, C_B), 0.02),
        'conv_ln_b': nrm(ks[18], (N_EVEN, C_B), 0.02),
        'w_out_ab': nrm(ks[19], (N_EVEN, C_A + C_B, d), (C_A + C_B) ** -0.5 * DEEPNORM_BETA),
        'w_qkv': nrm(ks[20], (N_ODD, d, 3 * d), d ** -0.5),
        'rpb': nrm(ks[21], (N_ODD, N_HEADS_C, 2 * WIN_ROWS - 1, 2 * WIN_COLS - 1), 0.1),
        'w_out_c': nrm(ks[22], (N_ODD, d, d), d ** -0.5 * DEEPNORM_BETA),
        'w_router': nrm(ks[23], (DEPTH, d, N_EXPERTS), d ** -0.5),
        'b_router': nrm(ks[24], (DEPTH, N_EXPERTS), 0.01),
        'w_gate_up': nrm(ks[25], (DEPTH, N_EXPERTS, d, 2 * D_EXPERT), d ** -0.5),
        'b_gate_up': nrm(ks[26], (DEPTH, N_EXPERTS, 2 * D_EXPERT), 0.01),
        'w_down': nrm(ks[27], (DEPTH, N_EXPERTS, D_EXPERT, d), D_EXPERT ** -0.5 * DEEPNORM_BETA),
        'b_down': nrm(ks[28], (DEPTH, N_EXPERTS, d), 0.01),
    }


def reference(x_prompt, x_sample, c, cache_k, cache_v, c_ctx, w_mod, b_mod, ln_g, ln_b,
              w_in_ab, sgu_ln_g, sgu_ln_b, w_spatial, b_spatial, conv_w, conv_b,
              conv_ln_g, conv_ln_b, w_out_ab, w_qkv, rpb, w_out_c, w_router, b_router,
              w_gate_up, b_gate_up, w_down, b_down):

    def run(x, cvec, ctx_kv):
        ks_out = []
        vs_out = []
        for l in range(DEPTH):
            i = l // 2
            mod = adaln(cvec, w_mod[l], b_mod[l])
            h = x * (1.0 + mod[:, 1]) + mod[:, 0]
            if l % 2 == 0:
                y = mixer_ab(h, w_in_ab[i], sgu_ln_g[i], sgu_ln_b[i], w_spatial[i], b_spatial[i],
                             conv_w[i], conv_b[i], conv_ln_g[i], conv_ln_b[i], w_out_ab[i])
            elif ctx_kv is None:
                y, k_l, v_l = attn_ctx(h, w_qkv[i], w_out_c[i])
                ks_out.append(k_l)
                vs_out.append(v_l)
            else:
                y = attn_latent(h, ctx_kv[0][:, i], ctx_kv[1][:, i], w_qkv[i], rpb[i], w_out_c[i])
            x = layer_norm(DEEPNORM_ALPHA * x + mod[:, 2] * y, ln_g[l, 0], ln_b[l, 0])
            h = x * (1.0 + mod[:, 4]) + mod[:, 3]
            y = moe(h, w_router[l], b_router[l], w_gate_up[l], b_gate_up[l], w_down[l], b_down[l])
            x = layer_norm(DEEPNORM_ALPHA * x + mod[:, 5] * y, ln_g[l, 1], ln_b[l, 1])
        return x, ks_out, vs_out

    y_prompt, ks_p, vs_p = run(x_prompt, c_ctx[None, :], None)
    new_cache_k = jnp.stack(ks_p, axis=1)
    new_cache_v = jnp.stack(vs_p, axis=1)
    y_sample, _, _ = run(x_sample, c, (cache_k, cache_v))
    return (y_prompt, y_sample, new_cache_k, new_cache_v)
```

```python
import contextlib
import numpy as np
import concourse.bass as bass
import concourse.mybir as mybir
from concourse.bass_utils import run_bass_kernel_spmd

F32 = mybir.dt.float32
BF16 = mybir.dt.bfloat16
I32 = mybir.dt.int32
ALU = mybir.AluOpType
AF = mybir.ActivationFunctionType
AX = mybir.AxisListType

import os
VDBG = int(os.environ.get("VDBG", "0"))
NCORES = 2
NV = 4
D = 1024
NT = 12
NTOK = NT * 128
DEPTH = 4
NE = 32
TPE = 4
CAP = TPE * 128
NSLOT = NE * CAP
ALPHA = (2 * DEPTH) ** 0.25
EPS = 1e-5
GELU_C = 1.5957691216057308


class StopBuild(Exception):
    pass


class Buf:
    __slots__ = ("name", "w", "r", "base")

    def __init__(self, name):
        self.name = name
        self.w = {}
        self.r = {}
        self.base = {}


class Sched:
    ENGS = ("pe", "act", "dve", "pool", "sp")

    def __init__(self, nc):
        self.nc = nc
        self.prog = {e: [] for e in self.ENGS}
        self.cnt = {e: 0 for e in ("pe", "act", "dve", "pool")}
        self.known = {e: {} for e in self.ENGS}
        self.dma_tot = {}
        self.out_toks = []

    def op(self, eng, fn, reads=(), writes=(), pwrites=(), dma=None, is_out=False, dma_inc=16):
        waits = {}
        known = self.known[eng]

        def need(key, n):
            if key == ("c", "pe") and eng == "pe" and dma is None:
                return
            if known.get(key, 0) >= n:
                return
            if waits.get(key, 0) < n:
                waits[key] = n

        for b in reads:
            for k, n in b.w.items():
                need(k, n)
        for b in writes:
            for k, n in b.w.items():
                need(k, n)
            for k, n in b.r.items():
                need(k, n)
        for b in pwrites:
            for k, n in b.r.items():
                need(k, n)
            for k, n in b.base.items():
                need(k, n)
        if dma is not None:
            prev = self.dma_tot.get(dma, 0)
            if prev:
                need(("d", dma), prev)
        for k, n in waits.items():
            known[k] = n
        if dma is not None:
            tot = self.dma_tot.get(dma, 0) + dma_inc
            self.dma_tot[dma] = tot
            tok = (("d", dma), tot)
        else:
            self.cnt[eng] += 1
            tok = (("c", eng), self.cnt[eng])
        self.prog[eng].append((list(waits.items()), fn, tok, dma_inc))
        for b in reads:
            if b.r.get(tok[0], 0) < tok[1]:
                b.r[tok[0]] = tok[1]
        for b in writes:
            b.w = {tok[0]: tok[1]}
            b.base = {tok[0]: tok[1]}
            b.r = {}
        for b in pwrites:
            b.w[tok[0]] = tok[1]
        if is_out:
            self.out_toks.append(tok)
        return tok

    def barrier(self):
        allt = {("c", e): c for e, c in self.cnt.items() if c}
        for k, n in self.dma_tot.items():
            allt[("d", k)] = n
        for eng in self.ENGS:
            waits = []
            for k, n in allt.items():
                if k == ("c", "pe") and eng == "pe":
                    continue
                if self.known[eng].get(k, 0) < n:
                    waits.append((k, n))
                    self.known[eng][k] = n
            if waits:
                self.prog[eng].append((waits, None, None, 0))

    def emit(self, st):
        nc = self.nc
        sems = {}
        for e in self.cnt:
            sems[("c", e)] = st.enter_context(nc.semaphore(f"s_{e}"))
        for i, k in enumerate(self.dma_tot):
            sems[("d", k)] = st.enter_context(nc.semaphore(f"d{i}"))
        final_waits = {}
        for e, c in self.cnt.items():
            if c:
                final_waits[("c", e)] = c
        for k, n in self.dma_tot.items():
            final_waits[("d", k)] = n
        block = st.enter_context(nc.Block())
        prog = self.prog

        def mk(engname):
            def body(e):
                for waits, fn, tok, dinc in prog[engname]:
                    for key, n in waits:
                        e.wait_ge(sems[key], n)
                    if fn is None:
                        continue
                    ins = fn(e)
                    if tok[0][0] == "c":
                        ins.then_inc(sems[tok[0]], 1)
                    else:
                        ins.then_inc(sems[tok[0]], dinc)
                if engname == "sp":
                    for key, n in final_waits.items():
                        e.wait_ge(sems[key], n)
            return body

        block.tensor(mk("pe"))
        block.scalar(mk("act"))
        block.vector(mk("dve"))
        block.gpsimd(mk("pool"))
        block.sync(mk("sp"))


TQ_CH = {0: (0, 6), 1: (1, 6), 2: (2, 7), 3: (3, 8), 4: (4, 9), 5: (5, 10), 6: (6, 11), 7: (6, 12)}


def _attn_tt(rpb_i):
    qc = np.arange(64)
    kc = np.arange(64)
    cs = np.clip(qc - 8, 0, 48)
    colv = (kc[None, :] >= cs[:, None]) & (kc[None, :] < cs[:, None] + 16)
    dc = np.clip(kc[None, :] - qc[:, None] + 15, 0, 30)
    out = np.full((16, 2, 64, 16, 64), np.float32(-1e30), np.float32)
    for qi in range(2):
        for d in range(16):
            dr = d - 8 - qi
            if -7 <= dr <= 7:
                g = rpb_i[:, dr + 7][:, dc]
                out[:, qi, :, d, :] = np.where(colv[None], g, np.float32(-1e30))
    return out.reshape(16, 128, 16, 64)


def _rowmask(j):
    m = np.full((2, 8, 24), np.float32(-1e30), np.float32)
    for tq in range(8):
        for qi in range(2):
            r = 16 * j + 2 * tq + qi
            rs = min(max(r - 4, 0), 56)
            for w in range(24):
                rp = 16 * j - 4 + w
                if 0 <= rp < 64 and rs <= rp < rs + 8:
                    m[qi, tq, w] = 0.0
    return np.ascontiguousarray(np.repeat(m[:, None], 64, axis=1).reshape(128, 8, 24))


def _make_input(name, inp, c):
    f = np.float32
    g = lambda k: np.asarray(inp[k], f)
    if name == "xin":
        out = np.empty((NV, NTOK, D), f)
        for v in range(NV):
            out[v, 0:256] = inp["x_prompt"][8 * c + 2 * v]
            out[v, 256:512] = inp["x_prompt"][8 * c + 2 * v + 1]
            out[v, 512:] = inp["x_sample"][c, 1024 * v:1024 * (v + 1)]
        return out
    if name == "cv":
        cv2 = np.stack([g("c_ctx"), g("c")[c]], 0)
        return cv2.reshape(2, 8, 128).transpose(2, 1, 0)
    if name == "ckT":
        return g("cache_k")[c].reshape(2, 256, 1024).transpose(0, 2, 1)
    if name == "cv_v":
        return g("cache_v")[c].reshape(2, 256, 1024)
    if name == "tt":
        return np.stack([_attn_tt(g("rpb")[ii]) for ii in range(2)], 0)
    if name == "rowmask":
        return np.stack([_rowmask(j) for j in range(NV)], 0)
    direct = {"w_mod": "w_mod", "w_in": "w_in_ab", "w_oab": "w_out_ab", "w_qkv": "w_qkv", "w_oc": "w_out_c", "w_gu": "w_gate_up",
              "w_dn": "w_down", "b_mod": "b_mod", "b_gu": "b_gate_up", "b_dn": "b_down", "b_router": "b_router",
              "sgu_g": "sgu_ln_g", "sgu_b": "sgu_ln_b", "bsp": "b_spatial"}
    if name in direct:
        return g(direct[name])
    if name == "ln_g":
        return g("ln_g").reshape(8, 1024)
    if name == "ln_b":
        return g("ln_b").reshape(8, 1024)
    if name == "bmodT":
        return g("b_mod")[:, :2048].reshape(4, 16, 128).transpose(0, 2, 1)
    if name == "w_r":
        return g("w_router").reshape(4, 8, 128, 32).transpose(0, 2, 1, 3)
    if name == "wspT":
        return g("w_spatial").transpose(0, 1, 3, 2)
    if name == "cwT":
        return g("conv_w").transpose(0, 2, 1).reshape(2, 4, 128, 31).transpose(0, 2, 1, 3)
    if name in ("cb", "clg", "clb"):
        src = {"cb": "conv_b", "clg": "conv_ln_g", "clb": "conv_ln_b"}[name]
        return g(src).reshape(2, 4, 128).transpose(0, 2, 1)
    if name == "ident":
        return np.eye(128, dtype=f)
    if name == "tri":
        return np.triu(np.ones((128, 128), f), 1)
    if name == "ecap":
        return np.broadcast_to((np.arange(NE) * CAP).astype(f)[None, :], (128, NE))
    raise KeyError(name)


def _in_maps(inp, decl):
    maps = []
    for i in range(NCORES):
        m = {}
        for name, (shape, dt) in decl.items():
            a = np.ascontiguousarray(_make_input(name, inp, i))
            assert tuple(a.shape) == tuple(shape), (name, a.shape, shape)
            m[name] = a
        maps.append(m)
    return maps


def build(nlayers=DEPTH, dbg_stage=None, nv=NV):
    nc = bass.Bass("TRN2", target_bir_lowering=False)
    decl = {}

    def din(n, s, dt=F32):
        decl[n] = (tuple(s), dt)
        return nc.dram_tensor(n, list(s), dt, kind="ExternalInput").ap()
    dout = lambda n, s: nc.dram_tensor(n, list(s), F32, kind="ExternalOutput").ap()
    dscr = lambda n, s, dt: nc.dram_tensor(n, list(s), dt).ap()
    _lazy = {}

    def lazy(n, s, dt=F32):
        if n not in _lazy:
            _lazy[n] = din(n, s, dt)
        return _lazy[n]

    xin = din("xin", [NV, NTOK, D]); cv = din("cv", [128, 8, 2])
    b_mod = din("b_mod", [4, 6144]); bmodT = din("bmodT", [4, 128, 16]); w_mod = din("w_mod", [4, 1024, 6144])
    ln_g = din("ln_g", [8, 1024]); ln_b = din("ln_b", [8, 1024])
    ident_d = din("ident", [128, 128]); tri_d = din("tri", [128, 128]); ecap_d = din("ecap", [128, NE])
    xout = dout("xout", [NV, NTOK, D]); nck = dout("nck", [NV, 2, 2, 256, 1024]); ncv = dout("ncv", [NV, 2, 2, 256, 1024])
    XS = dscr("XS", [2, NV, NTOK, D], F32)
    xdisp = dscr("xdisp", [NSLOT, D], BF16); ybuf = dscr("ybuf", [NSLOT, D], F32)
    QT = dscr("QT", [1024, NTOK], BF16); KT = dscr("KT", [1024, NTOK + 512], BF16); Vd = dscr("Vd", [NTOK + 512, 1024], BF16)
    bXS = [[Buf(f"XS{a}_{v}") for v in range(NV)] for a in range(2)]
    bxdisp = Buf("xdisp"); bybuf = Buf("ybuf"); bQT = Buf("QT"); bKT = Buf("KT"); bVd = Buf("Vd")

    S = Sched(nc)
    with contextlib.ExitStack() as st:
        sb = lambda n, s, dt=F32: st.enter_context(nc.sbuf_tensor("sb_" + n, list(s), dt))
        V = lambda fn, r=(), w=(), pw=(): S.op("dve", fn, r, w, pw)
        A = lambda fn, r=(), w=(), pw=(): S.op("act", fn, r, w, pw)
        G = lambda fn, r=(), w=(), pw=(): S.op("pool", fn, r, w, pw)
        T = lambda fn, r=(), w=(), pw=(): S.op("pe", fn, r, w, pw)
        DS = lambda key, fn, r=(), w=(), pw=(), out=False: S.op("sp", fn, r, w, pw, dma=key, is_out=out)
        DP = lambda key, fn, r=(), w=(), pw=(), inc=16: S.op("pool", fn, r, w, pw, dma=key, dma_inc=inc)

        xres = sb("xres", [128, NT, D]); bx = [Buf(f"x{t}") for t in range(NT)]
        ident = sb("ident", [128, 128]); identb = sb("identb", [128, 128], BF16); tri = sb("tri", [128, 128], BF16)
        onesb = sb("onesb", [128, 128], BF16); onesf = sb("onesf", [128, 128]); ecap = sb("ecap", [128, NE])
        scT = sb("scT", [128, 8, 2], BF16); screp = sb("screp", [128, 2, 8, 128], BF16)
        modT = sb("modT", [128, 2, 8, 2])
        bconst = Buf("const")
        NRING = 4
        wring = [sb(f"wring{i}", [128, 8, 512], BF16) for i in range(NRING)]
        bring = [Buf(f"ring{i}") for i in range(NRING)]
        ring_ctr = [0]
        ARENA = 114 * 1024
        hb_t = sb("hb_t", [128, 2, 1024], BF16); desti_t = sb("desti_t", [128, NT, 4], I32)
        arena = sb("arena", [128, ARENA // 4])
        apos = [0]

        def carve(shape, dt=F32):
            esz = 2 if dt == BF16 else 4
            n = int(np.prod(shape)) * esz
            n4 = (n + 3) // 4
            if apos[0] % 8:
                apos[0] += 8 - apos[0] % 8
            v = arena[:, apos[0]:apos[0] + n4]
            apos[0] += n4 + (n4 % 2)
            assert apos[0] * 4 <= ARENA, (apos[0] * 4, ARENA)
            if dt != F32:
                v = v.bitcast(dt)
            if len(shape) == 2:
                return v.rearrange("p (a b) -> p a b", a=shape[0])
            if len(shape) == 3:
                return v.rearrange("p (a b c) -> p a b c", a=shape[0], b=shape[1])
            return v

        def new_phase():
            S.barrier()
            apos[0] = 0

        def subphase(mark):
            S.barrier()
            apos[0] = mark

        psum = [st.enter_context(nc.psum_tensor(f"ps{i}", [128, 512], F32)) for i in range(8)]
        pb = [Buf(f"ps{i}") for i in range(8)]
        psb = lambda i: psum[i][:].bitcast(BF16)

        stage = carve([128])
        DS("c0", lambda e: e.dma_start(out=ident[:], in_=ident_d), w=[bconst])
        V(lambda e: e.tensor_copy(out=identb[:], in_=ident[:]), [bconst], pw=[bconst])
        tmpb = Buf("tmp")
        DS("c1", lambda e: e.dma_start(out=stage, in_=tri_d), w=[tmpb])
        V(lambda e: e.tensor_copy(out=tri[:], in_=stage), [tmpb], pw=[bconst])
        V(lambda e: e.memset(onesb[:], 1.0), pw=[bconst])
        V(lambda e: e.memset(onesf[:], 1.0), pw=[bconst])
        DS("c2", lambda e: e.dma_start(out=ecap[:], in_=ecap_d), pw=[bconst])
        cvs = carve([8, 2])
        bcv = Buf("cv")
        DS("c5", lambda e: e.dma_start(out=cvs, in_=cv), w=[bcv])
        A(lambda e: e.activation(out=scT[:], in_=cvs, func=AF.Silu), [bcv], pw=[bconst])
        for s_ in range(2):
            for kc in range(8):
                V(lambda e, s_=s_, kc=kc: e.tensor_scalar(out=screp[:, s_, kc, :], in0=onesb[:], scalar1=scT[:, kc, s_:s_ + 1],
                                                         scalar2=None, op0=ALU.mult), [bconst], pw=[bconst])

        zt = carve([4, 1024], BF16); bzt = Buf("zt")
        V(lambda e: e.memset(zt, 0.0), w=[bzt])
        xdv = xdisp.rearrange("(a p) d -> p a d", p=128)
        for a in range(0, NSLOT // 128, 4):
            DS(f"z{(a // 4) % 4}", lambda e, a=a: e.dma_start(out=xdv[:, a:a + 4, :], in_=zt), [bzt], w=[bxdisp] if a == 0 else (), pw=() if a == 0 else [bxdisp])

        _regs = {}

        def getreg(e, val):
            if val not in _regs:
                _regs[val] = e.to_reg(val)
            return _regs[val]

        def rows8(ap2d):
            return ap2d.rearrange("(c p) n -> p c n", p=128)

        def wload(srcs):
            i = ring_ctr[0] % NRING
            ring_ctr[0] += 1
            for q, (src, c0) in enumerate(srcs):
                n = src.shape[2]
                fn = lambda e, i=i, src=src, c0=c0, n=n: e.dma_start(out=wring[i][:, :, c0:c0 + n], in_=src)
                if q == 0:
                    DP(f"w{i}_{q}", fn, w=[bring[i]])
                else:
                    DP(f"w{i}_{q}", fn, pw=[bring[i]])
            return wring[i], bring[i]

        def mm8(out, lhs_fn, rhs_fn, nk=8):
            def fn(e):
                ins = None
                for kc in range(nk):
                    ins = e.matmul(out, lhsT=lhs_fn(kc), rhs=rhs_fn(kc), start=(kc == 0), stop=(kc == nk - 1))
                return ins
            return fn

        def bload(key, dst, src_row, bufobj, pw=False):
            if pw:
                DS(key, lambda e: e.dma_start(out=dst, in_=src_row.partition_broadcast(128)), pw=[bufobj])
            else:
                DS(key, lambda e: e.dma_start(out=dst, in_=src_row.partition_broadcast(128)), w=[bufobj])

        def layer_norm(u, width, gbc, bbc, out, rb, wb, tmp):
            nchunk = width // 512
            for c in range(nchunk):
                V(lambda e, c=c: e.bn_stats(out=tmp[:, c * 6:(c + 1) * 6], in_=u[:, c * 512:(c + 1) * 512]), rb, pw=[wb] if c else (), w=[wb] if not c else ())
            V(lambda e: e.bn_aggr(out=tmp[:, 12:14], in_=tmp[:, 0:6 * nchunk]), [wb], pw=[wb])
            V(lambda e: e.tensor_scalar(out=tmp[:, 14:15], in0=tmp[:, 13:14], scalar1=EPS, scalar2=None, op0=ALU.add), [wb], pw=[wb])
            A(lambda e: e.sqrt(out=tmp[:, 14:15], in_=tmp[:, 14:15]), [wb], pw=[wb])
            V(lambda e: e.reciprocal(out=tmp[:, 15:16], in_=tmp[:, 14:15]), [wb], pw=[wb])
            V(lambda e: e.tensor_scalar(out=out, in0=u, scalar1=tmp[:, 12:13], scalar2=tmp[:, 15:16], op0=ALU.subtract, op1=ALU.mult), list(rb) + [wb], pw=[wb])
            V(lambda e: e.tensor_tensor(out=out, in0=out, in1=gbc, op=ALU.mult), [wb], pw=[wb])
            V(lambda e: e.tensor_tensor(out=out, in0=out, in1=bbc, op=ALU.add), [wb], pw=[wb])

        def gelu_from(src, dst, t1, t2, r, bs, bd):
            A(lambda e: e.activation(out=t1, in_=src, func=AF.Square), r, w=[bs])
            V(lambda e: e.tensor_scalar(out=t1, in0=t1, scalar1=0.044715, scalar2=1.0, op0=ALU.mult, op1=ALU.add), [bs], pw=[bs])
            V(lambda e: e.tensor_tensor(out=t1, in0=t1, in1=src, op=ALU.mult), list(r) + [bs], pw=[bs])
            A(lambda e: e.activation(out=t2, in_=t1, func=AF.Sigmoid, scale=GELU_C), [bs], pw=[bs])
            V(lambda e: e.tensor_tensor(out=dst, in0=t2, in1=src, op=ALU.mult), list(r) + [bs], pw=[bs, bd])

        SETOF = lambda t: 0 if t < 4 else 1
        mbias = [None]

        def mod_rows(l, which_list, dsts, bdst):
            for wi, which in enumerate(which_list):
                for half in range(2):
                    c = which * 2 + half
                    wt, wbuf = wload([(rows8(w_mod[l][:, c * 512:(c + 1) * 512]), 0)])
                    bb = mbias[0][half % 2]
                    bload(f"mb{half}", bb[0], b_mod[l:l + 1, c * 512:(c + 1) * 512], bb[1])
                    for s_ in range(2):
                        bank = s_
                        T(mm8(psum[bank][:], lambda kc, s_=s_: screp[:, s_, kc, :], lambda kc, wt=wt: wt[:, kc, :]),
                          [wbuf, bconst], [pb[bank]])
                        dst = dsts[wi][s_][:, half * 512:(half + 1) * 512]
                        V(lambda e, bank=bank, dst=dst, bb=bb: e.tensor_tensor(out=dst, in0=psum[bank][:], in1=bb[0], op=ALU.add),
                          [pb[bank], bb[1]], pw=[bdst])
                        if which in (1, 4):
                            V(lambda e, dst=dst: e.tensor_scalar(out=dst, in0=dst, scalar1=1.0, scalar2=None, op0=ALU.add), [bdst], pw=[bdst])

        def mod_cols(l, bdst):
            bmt = carve([16])
            bbm = Buf("bmt")
            DS("bmt", lambda e: e.dma_start(out=bmt, in_=bmodT[l]), w=[bbm])
            for c in range(4):
                wt, wbuf = wload([(rows8(w_mod[l][:, c * 512:(c + 1) * 512]), 0)])
                for fo in range(4):
                    q = c * 4 + fo
                    T(mm8(psum[6][:, 0:2], lambda kc, wt=wt, fo=fo: wt[:, kc, fo * 128:(fo + 1) * 128], lambda kc: scT[:, kc, :]),
                      [wbuf, bconst], [pb[6]])
                    add1 = 1.0 if q >= 8 else 0.0
                    V(lambda e, q=q, add1=add1: e.tensor_scalar(out=modT[:, q // 8, q % 8, :], in0=psum[6][:, 0:2], scalar1=bmt[:, q:q + 1],
                                                                scalar2=add1, op0=ALU.add, op1=ALU.add), [pb[6], bbm], pw=[bdst])

        def transpose_mod(src_tile_fn, rb, dst_fn, s_, bmodc, bdst):
            for half in range(2):
                bank = 4 + half

                def trs(e, half=half, bank=bank):
                    ins = None
                    for q in range(4):
                        ins = e.transpose(out=psum[bank][:, q * 128:(q + 1) * 128], in_=src_tile_fn(half * 4 + q), identity=ident[:])
                    return ins
                T(trs, list(rb) + [bconst], [pb[bank]])
                for q in range(4):
                    kc = half * 4 + q
                    A(lambda e, kc=kc, q=q, bank=bank: e.activation(out=dst_fn(kc), in_=psum[bank][:, q * 128:(q + 1) * 128], func=AF.Identity,
                                                                    scale=modT[:, 1, kc, s_:s_ + 1], bias=modT[:, 0, kc, s_:s_ + 1]), [pb[bank], bmodc], pw=[bdst])

        def post_mixer(ln_idx, prep, wsrc, gate_bc, bgate):
            lg = carve([1024]); lb = carve([1024]); blg = Buf("lg"); blb = Buf("lb")
            bload("lng", lg, ln_g[ln_idx:ln_idx + 1, :], blg)
            bload("lnb", lb, ln_b[ln_idx:ln_idx + 1, :], blb)
            ws = [wload([(rows8(wsrc[:, 0:512]), 0)]), wload([(rows8(wsrc[:, 512:1024]), 0)])]
            ut = [carve([1024]), carve([1024])]; but = [Buf("u0"), Buf("u1")]
            tmp = [carve([16]), carve([16])]
            for t in range(NT):
                s_ = SETOF(t)
                u = ut[t % 2]; bu = but[t % 2]
                lfn, lb_ = prep(t)
                for nb in range(2):
                    bank = nb
                    T(mm8(psum[bank][:], lfn, lambda kc, nb=nb: ws[nb][0][:, kc, :]), list(lb_) + [ws[nb][1]], [pb[bank]])
                    usl = u[:, nb * 512:(nb + 1) * 512]
                    V(lambda e, bank=bank, usl=usl, s_=s_, nb=nb: e.tensor_tensor(out=usl, in0=psum[bank][:], in1=gate_bc[s_][:, nb * 512:(nb + 1) * 512], op=ALU.mult),
                      [pb[bank], bgate], w=[bu] if nb == 0 else (), pw=[bu] if nb else ())
                V(lambda e, t=t, u=u: e.scalar_tensor_tensor(out=u, in0=xres[:, t, :], scalar=ALPHA, in1=u, op0=ALU.mult, op1=ALU.add), [bx[t], bu], pw=[bu])
                layer_norm(u, 1024, lg, lb, xres[:, t, :], [bu, blg, blb], bx[t], tmp[t % 2])

        def make_hT(bmodc):
            hT = carve([8, NTOK], BF16); bhT = Buf("hT")
            for t in range(NT):
                transpose_mod(lambda kc, t=t: xres[:, t, kc * 128:(kc + 1) * 128], [bx[t]], lambda kc, t=t: hT[:, kc, t * 128:(t + 1) * 128], SETOF(t), bmodc, bhT)
            return hT, bhT

        def mixer_even(l, i2, v, src, bsrc, bmodc, gate1, bgate1):
            w_in = lazy("w_in", [2, 1024, 2048]); w_oab = lazy("w_oab", [2, 1024, 1024])
            sgu_g = lazy("sgu_g", [2, 512]); sgu_b = lazy("sgu_b", [2, 512]); wspT = lazy("wspT", [2, 8, 128, 128]); bsp = lazy("bsp", [2, 8, 128])
            cwT = lazy("cwT", [2, 128, 4, 31]); cb = lazy("cb", [2, 128, 4]); clg = lazy("clg", [2, 128, 4]); clb = lazy("clb", [2, 128, 4])
            catT = carve([8, NTOK], BF16); bcat = Buf("catT")
            uT = carve([4, NTOK], BF16); buT = Buf("uT")
            GLW = 286 * 2 + 1054
            glT = carve([4, GLW], BF16); bgl = Buf("glT")
            seg_off = [15, 286 + 15, 572 + 15]
            so = seg_off[2]
            mark_h = apos[0]
            hT, bhT = make_hT(bmodc)
            mark = apos[0]
            sgT = carve([4, NTOK], BF16); bsg = Buf("sgT")
            t1 = [carve([512]), carve([512])]; t2 = [carve([512]), carve([512])]; bt = [Buf("t0"), Buf("t1")]
            xh = carve([1024]); bxh = Buf("xh"); hTh = carve([8, 128], BF16); bhTh = Buf("hTh"); sgh = carve([4, 128], BF16)
            V(lambda e: e.memset(glT, 0.0), w=[bgl])
            V(lambda e: e.memset(xh, 0.0), w=[bxh])
            if v > 0:
                DS("xh0", lambda e: e.dma_start(out=xh[0:15, :], in_=src[v - 1][NTOK - 15:NTOK, :]), [bsrc[v - 1]], pw=[bxh])
            if v < NV - 1:
                DS("xh1", lambda e: e.dma_start(out=xh[32:47, :], in_=src[v + 1][512:527, :]), [bsrc[v + 1]], pw=[bxh])
            transpose_mod(lambda kc: xh[:, kc * 128:(kc + 1) * 128], [bxh], lambda kc: hTh[:, kc, :], 1, bmodc, bhTh)

            def tok2gl(tb):
                out = []
                for t in range(tb * 4, tb * 4 + 4):
                    seg = 0 if t < 2 else (1 if t < 4 else 2)
                    base = [0, 256, 512][seg]
                    out.append((t * 128, 128, seg_off[seg] + t * 128 - base))
                return out
            for which, col0 in (("u", 0), ("g", 1536), ("a", 1024)):
                wt, wbuf = wload([(rows8(w_in[i2][:, col0:col0 + 512]), 0)])
                for fo in range(4):
                    for tb in range(3):
                        k = (fo * 3 + tb) % 2
                        T(mm8(psum[k][:], lambda kc, wt=wt, fo=fo: wt[:, kc, fo * 128:(fo + 1) * 128],
                              lambda kc, tb=tb: hT[:, kc, tb * 512:(tb + 1) * 512]), [wbuf, bhT], [pb[k]])
                        if which == "u":
                            gelu_from(psum[k][:], uT[:, fo, tb * 512:(tb + 1) * 512], t1[k], t2[k], [pb[k]], bt[k], buT)
                        elif which == "g":
                            A(lambda e, k=k, fo=fo, tb=tb: e.activation(out=sgT[:, fo, tb * 512:(tb + 1) * 512], in_=psum[k][:], func=AF.Sigmoid),
                              [pb[k]], pw=[bsg])
                        else:
                            for (tok0, n, gc) in tok2gl(tb):
                                o = tok0 - tb * 512
                                V(lambda e, k=k, fo=fo, o=o, n=n, gc=gc, tok0=tok0: e.tensor_tensor(
                                    out=glT[:, fo, gc:gc + n], in0=psum[k][:, o:o + n], in1=sgT[:, fo, tok0:tok0 + n], op=ALU.mult),
                                  [pb[k], bsg], pw=[bgl])
                    if which in ("g", "a"):
                        T(mm8(psum[2][:, 0:128], lambda kc, wt=wt, fo=fo: wt[:, kc, fo * 128:(fo + 1) * 128], lambda kc: hTh[:, kc, :]), [wbuf, bhTh], [pb[2]])
                        if which == "g":
                            A(lambda e, fo=fo: e.activation(out=sgh[:, fo, :], in_=psum[2][:, 0:128], func=AF.Sigmoid), [pb[2]], pw=[bsg])
                        else:
                            if v > 0:
                                V(lambda e, fo=fo: e.tensor_tensor(out=glT[:, fo, so - 15:so], in0=psum[2][:, 0:15], in1=sgh[:, fo, 0:15], op=ALU.mult), [pb[2], bsg], pw=[bgl])
                            if v < NV - 1:
                                V(lambda e, fo=fo: e.tensor_tensor(out=glT[:, fo, so + 1024:so + 1039], in0=psum[2][:, 32:47], in1=sgh[:, fo, 32:47], op=ALU.mult), [pb[2], bsg], pw=[bgl])
            subphase(mark)
            vn = carve([NT, 512], BF16); bvn = Buf("vn")
            t1v = carve([512]); t2v = carve([512]); btv = Buf("tv")
            sg_ = carve([512]); sb_ = carve([512]); bsgu = Buf("sgu")
            bload("sg0", sg_, sgu_g[i2:i2 + 1, :], bsgu)
            bload("sg1", sb_, sgu_b[i2:i2 + 1, :], bsgu, pw=True)
            wt, wbuf = wload([(rows8(w_in[i2][:, 512:1024]), 0)])
            vg = carve([512]); bvg = Buf("vg"); vo = carve([512]); tmpv = carve([16])
            wsp = carve([8, 128], BF16); bwsp = Buf("wsp")
            DP("wsp", lambda e: e.dma_start(out=wsp, in_=wspT[i2].rearrange("g q p -> q g p")), w=[bwsp])
            bspr = carve([8, 128], BF16); bbsp = Buf("bsp")
            DP("bsp", lambda e: e.dma_start(out=bspr[0:1], in_=bsp[i2:i2 + 1]), w=[bbsp])
            for t in range(NT):
                k = t % 2
                T(mm8(psum[2 + k][:], lambda kc, t=t: hT[:, kc, t * 128:(t + 1) * 128], lambda kc, wt=wt: wt[:, kc, :]), [wbuf, bhT], [pb[2 + k]])
                gelu_from(psum[2 + k][:], vg, t1v, t2v, [pb[2 + k]], btv, bvg)
                layer_norm(vg, 512, sg_, sb_, vo, [bvg, bsgu], bvg, tmpv)
                A(lambda e, t=t: e.copy(out=vn[:, t, :], in_=vo), [bvg], pw=[bvn])
            for t in range(NT):
                for fo in range(4):
                    for gi in range(2):
                        g_ = 2 * fo + gi
                        k = (fo * 2 + gi) % 2

                        def spmm(e, t=t, fo=fo, g_=g_, k=k):
                            e.matmul(psum[6 + k][:, 0:128], lhsT=vn[:, t, fo * 128:(fo + 1) * 128], rhs=wsp[:, g_, :], start=True, stop=False)
                            return e.matmul(psum[6 + k][:, 0:128], lhsT=onesb[0:1, :], rhs=bspr[0:1, g_, :], start=False, stop=True)
                        T(spmm, [bvn, bwsp, bbsp, bconst], [pb[6 + k]])
                        lo = gi * 64
                        V(lambda e, t=t, fo=fo, lo=lo, k=k: e.tensor_tensor(out=catT[lo:lo + 64, fo, t * 128:(t + 1) * 128], in0=psum[6 + k][lo:lo + 64, 0:128],
                                                                           in1=uT[lo:lo + 64, fo, t * 128:(t + 1) * 128], op=ALU.mult), [pb[6 + k], buT], pw=[bcat])
            subphase(mark_h)
            cw = carve([4, 31]); bcw = Buf("cw"); cbt = carve([4]); clgt = carve([4]); clbt = carve([4])
            DS("cw0", lambda e: e.dma_start(out=cw, in_=cwT[i2]), w=[bcw])
            DS("cw1", lambda e: e.dma_start(out=cbt, in_=cb[i2]), pw=[bcw])
            DS("cw2", lambda e: e.dma_start(out=clgt, in_=clg[i2]), pw=[bcw])
            DS("cw3", lambda e: e.dma_start(out=clbt, in_=clb[i2]), pw=[bcw])
            diag = [carve([31, 128], BF16), carve([31, 128], BF16)]; bdiag = [Buf("diag0"), Buf("diag1")]
            dcT = carve([4, 512]); bdc = Buf("dcT"); sq = [carve([512]), carve([512])]; bsq = [Buf("sq0"), Buf("sq1")]
            mr = carve([2, 512]); bmr = Buf("mr")
            blocks = [(0, 256, seg_off[0]), (256, 256, seg_off[1]), (512, 512, seg_off[2]), (1024, 512, seg_off[2] + 512)]
            inv512 = 1.0 / 512.0
            cnt = 0
            for (tok0, n, gc) in blocks:
                for fo in range(4):
                    k = fo % 2
                    dg = diag[cnt % 2]; bdg = bdiag[cnt % 2]; cnt += 1
                    for kk in range(31):
                        G(lambda e, fo=fo, kk=kk, dg=dg: e.tensor_scalar(out=dg[:, kk, :], in0=ident[:], scalar1=cw[:, fo, kk:kk + 1], scalar2=None, op0=ALU.mult),
                          [bcw, bconst], w=[bdg] if kk == 0 else (), pw=[bdg] if kk else ())

                    def cmm(e, fo=fo, n=n, gc=gc, k=k, dg=dg):
                        ins = None
                        for kk in range(31):
                            ins = e.matmul(psum[k][:, 0:n], lhsT=dg[:, kk, :], rhs=glT[:, fo, gc - 15 + kk:gc - 15 + kk + n], start=(kk == 0), stop=(kk == 30))
                        return ins
                    T(cmm, [bdg, bgl], [pb[k]])
                    A(lambda e, fo=fo, n=n, k=k: e.activation(out=dcT[:, fo, 0:n], in_=psum[k][:, 0:n], func=AF.Identity, bias=cbt[:, fo:fo + 1], scale=1.0),
                      [pb[k], bcw], w=[bdc] if fo == 0 else (), pw=[bdc] if fo else ())
                    A(lambda e, fo=fo, n=n, k=k: e.activation(out=sq[k][:, 0:n], in_=dcT[:, fo, 0:n], func=AF.Square), [bdc], w=[bsq[k]])
                    T(lambda e, fo=fo, n=n: e.matmul(psum[2][:, 0:n], lhsT=onesf[:], rhs=dcT[:, fo, 0:n], start=(fo == 0), stop=(fo == 3)), [bdc, bconst], [pb[2]])
                    T(lambda e, fo=fo, n=n, k=k: e.matmul(psum[3][:, 0:n], lhsT=onesf[:], rhs=sq[k][:, 0:n], start=(fo == 0), stop=(fo == 3)), [bsq[k], bconst], [pb[3]])
                V(lambda e, n=n: e.tensor_scalar(out=mr[:, 0, 0:n], in0=psum[2][:, 0:n], scalar1=inv512, scalar2=None, op0=ALU.mult), [pb[2]], w=[bmr])
                V(lambda e, n=n: e.tensor_tensor(out=mr[:, 1, 0:n], in0=mr[:, 0, 0:n], in1=mr[:, 0, 0:n], op=ALU.mult), [bmr], pw=[bmr])
                V(lambda e, n=n: e.scalar_tensor_tensor(out=mr[:, 1, 0:n], in0=psum[3][:, 0:n], scalar=inv512, in1=mr[:, 1, 0:n], op0=ALU.mult, op1=ALU.subtract), [pb[3], bmr], pw=[bmr])
                V(lambda e, n=n: e.tensor_scalar(out=mr[:, 1, 0:n], in0=mr[:, 1, 0:n], scalar1=EPS, scalar2=None, op0=ALU.add), [bmr], pw=[bmr])
                A(lambda e, n=n: e.sqrt(out=mr[:, 1, 0:n], in_=mr[:, 1, 0:n]), [bmr], pw=[bmr])
                V(lambda e, n=n: e.reciprocal(out=mr[:, 1, 0:n], in_=mr[:, 1, 0:n]), [bmr], pw=[bmr])
                for fo in range(4):
                    V(lambda e, fo=fo, n=n: e.tensor_tensor(out=dcT[:, fo, 0:n], in0=dcT[:, fo, 0:n], in1=mr[:, 0, 0:n], op=ALU.subtract), [bmr, bdc], pw=[bdc])
                    V(lambda e, fo=fo, n=n: e.tensor_tensor(out=dcT[:, fo, 0:n], in0=dcT[:, fo, 0:n], in1=mr[:, 1, 0:n], op=ALU.mult), [bmr, bdc], pw=[bdc])
                    A(lambda e, fo=fo, n=n, tok0=tok0: e.activation(out=catT[:, 4 + fo, tok0:tok0 + n], in_=dcT[:, fo, 0:n], func=AF.Silu,
                                                                    scale=clgt[:, fo:fo + 1], bias=clbt[:, fo:fo + 1]), [bdc, bcw], pw=[bcat])
            subphase(mark_h)
            post_mixer(2 * l, lambda t: ((lambda kc, t=t: catT[:, kc, t * 128:(t + 1) * 128]), [bcat]), w_oab[i2], gate1, bgate1)

        def mixer_odd(l, i2, v, src, bsrc, bmodc, gate1, bgate1):
            w_qkv = lazy("w_qkv", [2, 1024, 3072]); w_oc = lazy("w_oc", [2, 1024, 1024])
            ckT = lazy("ckT", [2, 1024, 256]); cvv = lazy("cv_v", [2, 256, 1024]); tt = lazy("tt", [2, 16, 128, 16, 64]); rowmask = lazy("rowmask", [NV, 128, 8, 24])
            mark = apos[0]
            hT, bhT = make_hT(bmodc)
            xh = carve([4, 1024]); bxh = Buf("xh"); hTh = carve([8, 512], BF16); bhTh = Buf("hTh")
            V(lambda e: e.memset(xh, 0.0), w=[bxh])
            if v > 0:
                DS("xh0", lambda e: e.dma_start(out=xh[:, 0:2, :], in_=src[v - 1][NTOK - 256:NTOK, :].rearrange("(a p) d -> p a d", p=128)), [bsrc[v - 1]], pw=[bxh])
            if v < NV - 1:
                DS("xh1", lambda e: e.dma_start(out=xh[:, 2:4, :], in_=src[v + 1][512:768, :].rearrange("(a p) d -> p a d", p=128)), [bsrc[v + 1]], pw=[bxh])
            for a in range(4):
                transpose_mod(lambda kc, a=a: xh[:, a, kc * 128:(kc + 1) * 128], [bxh], lambda kc, a=a: hTh[:, kc, a * 128:(a + 1) * 128], 1, bmodc, bhTh)
            if dbg_stage == ("odd", -3):
                raise StopBuild()
            stq = [carve([512], BF16), carve([512], BF16)]; bstq = [Buf("stq0"), Buf("stq1")]
            stf = [carve([512]), carve([512])]; bstf = [Buf("stf0"), Buf("stf1")]
            cnt = [0]

            def emit_fm(dst_ap, fo_rows, wt, wbuf, rhs, brhs, scale, bdst, first):
                k = cnt[0] % 2; cnt[0] += 1
                n = rhs(0).shape[1]
                T(mm8(psum[k][:, 0:n], lambda kc: wt[:, kc, fo_rows[0]:fo_rows[1]], rhs), [wbuf, brhs], [pb[k]])
                A(lambda e: e.activation(out=stq[k][:, 0:n], in_=psum[k][:, 0:n], func=AF.Identity, scale=scale), [pb[k]], w=[bstq[k]])
                DS(f"qk{k}", lambda e: e.dma_start(out=dst_ap, in_=stq[k][:, 0:n]), [bstq[k]], w=[bdst] if first else (), pw=() if first else [bdst])
            first = True
            for c in range(2):
                wt, wbuf = wload([(rows8(w_qkv[i2][:, c * 512:(c + 1) * 512]), 0)])
                for fo in range(4):
                    for tb in range(3):
                        emit_fm(QT[(c * 4 + fo) * 128:(c * 4 + fo + 1) * 128, tb * 512:(tb + 1) * 512], (fo * 128, fo * 128 + 128), wt, wbuf,
                                lambda kc, tb=tb: hT[:, kc, tb * 512:(tb + 1) * 512], bhT, 0.125, bQT, first)
                        first = False
            if dbg_stage == ("odd", -2):
                raise StopBuild()
            first = True
            for c in range(2):
                wt, wbuf = wload([(rows8(w_qkv[i2][:, 1024 + c * 512:1024 + (c + 1) * 512]), 0)])
                for fo in range(4):
                    r0 = (c * 4 + fo) * 128
                    for tb in range(3):
                        emit_fm(KT[r0:r0 + 128, tb * 512:(tb + 1) * 512], (fo * 128, fo * 128 + 128), wt, wbuf,
                                lambda kc, tb=tb: hT[:, kc, tb * 512:(tb + 1) * 512], bhT, 1.0, bKT, first)
                        first = False
                    emit_fm(KT[r0:r0 + 128, NTOK:NTOK + 512], (fo * 128, fo * 128 + 128), wt, wbuf, lambda kc: hTh[:, kc, :], bhTh, 1.0, bKT, False)
                for t in range(4):
                    k = cnt[0] % 2; cnt[0] += 1
                    T(mm8(psum[k][:], lambda kc, t=t: hT[:, kc, t * 128:(t + 1) * 128], lambda kc, wt=wt: wt[:, kc, :]), [wbuf, bhT], [pb[k]])
                    V(lambda e, k=k: e.tensor_copy(out=stf[k], in_=psum[k][:]), [pb[k]], w=[bstf[k]])
                    DS(f"nk{k}", lambda e, k=k, t=t, c=c: e.dma_start(out=nck[v, t // 2, i2, (t % 2) * 128:(t % 2 + 1) * 128, c * 512:(c + 1) * 512], in_=stf[k]), [bstf[k]], out=True)
            if dbg_stage == ("odd", -1):
                raise StopBuild()
            first = True
            for c in range(2):
                wt, wbuf = wload([(rows8(w_qkv[i2][:, 2048 + c * 512:2048 + (c + 1) * 512]), 0)])
                for t in range(NT + 4 if VDBG != 1 else NT):
                    k = cnt[0] % 2; cnt[0] += 1
                    if t < NT:
                        lf = lambda kc, t=t: hT[:, kc, t * 128:(t + 1) * 128]; bl = bhT
                    else:
                        lf = lambda kc, t=t: hTh[:, kc, (t - NT) * 128:(t - NT + 1) * 128]; bl = bhTh
                    T(mm8(psum[k][:], lf, lambda kc, wt=wt: wt[:, kc, :]), [wbuf, bl], [pb[k]])
                    V(lambda e, k=k: e.tensor_copy(out=stf[k], in_=psum[k][:]), [pb[k]], w=[bstf[k]])
                    A(lambda e, k=k: e.copy(out=stq[k], in_=stf[k]), [bstf[k]], w=[bstq[k]])
                    DS(f"qk{k}", lambda e, k=k, t=t, c=c: e.dma_start(out=Vd[t * 128:(t + 1) * 128, c * 512:(c + 1) * 512], in_=stq[k]), [bstq[k]], w=[bVd] if first else (), pw=() if first else [bVd])
                    first = False
                    if t < 4:
                        DS(f"nk{k}", lambda e, k=k, t=t, c=c: e.dma_start(out=ncv[v, t // 2, i2, (t % 2) * 128:(t % 2 + 1) * 128, c * 512:(c + 1) * 512], in_=stf[k]), [bstf[k]], out=True)
            if dbg_stage == ("odd", 0):
                raise StopBuild()
            subphase(mark)
            ao = carve([NT, 1024], BF16); bao = Buf("ao")
            mark2 = apos[0]
            Vw = carve([18, 1024], BF16); bVw = Buf("Vw")
            DS("vw0", lambda e: e.dma_start(out=Vw[:, 0:2, :], in_=Vd[NTOK:NTOK + 256, :].rearrange("(a p) d -> p a d", p=128)), [bVd], w=[bVw])
            DS("vw1", lambda e: e.dma_start(out=Vw[:, 2:10, :], in_=Vd[512:NTOK, :].rearrange("(a p) d -> p a d", p=128)), [bVd], pw=[bVw])
            DS("vw2", lambda e: e.dma_start(out=Vw[:, 10:12, :], in_=Vd[NTOK + 256:NTOK + 512, :].rearrange("(a p) d -> p a d", p=128)), [bVd], pw=[bVw])
            DS("vw3", lambda e: e.dma_start(out=Vw[:, 14:18, :], in_=Vd[0:512, :].rearrange("(a p) d -> p a d", p=128)), [bVd], pw=[bVw])
            DP("vw4", lambda e: e.dma_start(out=Vw[:, 12:14, :], in_=cvv[i2].rearrange("(a p) d -> p a d", p=128)), pw=[bVw])
            rm = carve([8, 24]); brm = Buf("rm")
            DS("rm", lambda e: e.dma_start(out=rm, in_=rowmask[v]), w=[brm])
            QTh = [carve([NTOK], BF16), carve([NTOK], BF16)]; Kown = [carve([NTOK], BF16), carve([NTOK], BF16)]
            Khal = [carve([512], BF16), carve([512], BF16)]; Kctx = [carve([256], BF16), carve([256], BF16)]
            tt2 = [carve([2, 16, 64], BF16), carve([2, 16, 64], BF16)]; bhp = [Buf("hp0"), Buf("hp1")]
            Ssb = carve([1024]); bS = Buf("S"); Pb = carve([1024], BF16); PT = carve([8, 128], BF16); bPT = Buf("PT")
            sm = carve([8]); bsm = Buf("sm")

            def softmax_pv(nk, vchunks, t, h, pl):
                V(lambda e: e.reduce_max(out=sm[:, 0:1], in_=Ssb[:, 0:nk], axis=AX.X), [bS], w=[bsm])
                V(lambda e: e.tensor_scalar(out=sm[:, 1:2], in0=sm[:, 0:1], scalar1=-1.0, scalar2=None, op0=ALU.mult), [bsm], pw=[bsm])
                A(lambda e: e.activation(out=Pb[:, 0:nk], in_=Ssb[:, 0:nk], func=AF.Exp, bias=sm[:, 1:2], scale=1.0, accum_out=sm[:, 2:3]), [bS, bsm], pw=[bsm, bS])
                V(lambda e: e.reciprocal(out=sm[:, 3:4], in_=sm[:, 2:3]), [bsm], pw=[bsm])
                nch = nk // 128

                def trs(e):
                    ins = None
                    for ci in range(nch):
                        ins = e.transpose(out=psb(4)[:, ci * 128:(ci + 1) * 128], in_=Pb[:, ci * 128:(ci + 1) * 128], identity=identb[:])
                    return ins
                T(trs, [bS, bconst], [pb[4]])
                A(lambda e: e.copy(out=PT[:, 0:nch, :], in_=psb(4)[:, 0:nch * 128].rearrange("p (a b) -> p a b", a=nch)), [pb[4]], w=[bPT])

                def pv(e):
                    ins = None
                    for ci, vc in enumerate(vchunks):
                        ins = e.matmul(psum[6][:, 0:64], lhsT=PT[:, ci, :], rhs=Vw[:, vc, h * 64:(h + 1) * 64], start=(ci == 0), stop=(ci == nch - 1))
                    return ins
                T(pv, [bPT, bVw], [pb[6]])
                V(lambda e: e.tensor_scalar(out=ao[:, t, h * 64:(h + 1) * 64], in0=psum[6][:, 0:64], scalar1=sm[:, 3:4], scalar2=None, op0=ALU.mult), [pb[6], bsm], pw=[bao])

            def kwin(kb, c):
                if c < 2:
                    return Khal[kb][:, c * 128:(c + 1) * 128]
                if c < 10:
                    return Kown[kb][:, 512 + (c - 2) * 128:512 + (c - 1) * 128]
                return Khal[kb][:, 256 + (c - 10) * 128:256 + (c - 9) * 128]
            for hp in range(8):
                kb = hp % 2
                r0 = hp * 128
                DS(f"hq{kb}", lambda e, kb=kb, r0=r0: e.dma_start(out=QTh[kb], in_=QT[r0:r0 + 128, :]), [bQT], w=[bhp[kb]])
                DS(f"hk{kb}", lambda e, kb=kb, r0=r0: e.dma_start(out=Kown[kb], in_=KT[r0:r0 + 128, 0:NTOK]), [bKT], pw=[bhp[kb]])
                DS(f"hh{kb}", lambda e, kb=kb, r0=r0: e.dma_start(out=Khal[kb], in_=KT[r0:r0 + 128, NTOK:NTOK + 512]), [bKT], pw=[bhp[kb]])
                DP(f"hc{kb}", lambda e, kb=kb, r0=r0: e.dma_start(out=Kctx[kb], in_=ckT[i2][r0:r0 + 128, :]), pw=[bhp[kb]])
                DP(f"ht{kb}", lambda e, kb=kb, hp=hp: e.dma_start(out=tt2[kb], in_=tt[i2][2 * hp:2 * hp + 2].rearrange("h p d k -> p h d k")), pw=[bhp[kb]])
                for hh in range(2):
                    h = 2 * hp + hh
                    pl = hh * 64
                    for t in range(4):
                        s0 = (t // 2) * 256
                        T(lambda e, kb=kb, pl=pl, t=t, s0=s0: e.matmul(psum[0][:, 0:256], lhsT=QTh[kb][pl:pl + 64, t * 128:(t + 1) * 128], rhs=Kown[kb][pl:pl + 64, s0:s0 + 256], start=True, stop=True),
                          [bhp[kb]], [pb[0]])
                        A(lambda e: e.copy(out=Ssb[:, 0:256], in_=psum[0][:, 0:256]), [pb[0]], w=[bS])
                        softmax_pv(256, [14 + 2 * (t // 2), 15 + 2 * (t // 2)], t, h, pl)
                    for tq in range(8):
                        t = 4 + tq
                        c0, c1 = TQ_CH[tq]
                        nloc = (c1 - c0) * 128

                        def sc(e, kb=kb, pl=pl, t=t, c0=c0, c1=c1):
                            q = QTh[kb][pl:pl + 64, t * 128:(t + 1) * 128]
                            ins = e.matmul(psum[0][:, 0:256], lhsT=q, rhs=Kctx[kb][pl:pl + 64, :], start=True, stop=True)
                            for i, c in enumerate(range(c0, c1)):
                                bank, off = (0, 256 + i * 128) if i < 2 else (1, (i - 2) * 128)
                                ins = e.matmul(psum[bank][:, off:off + 128], lhsT=q, rhs=kwin(kb, c)[pl:pl + 64, :], start=True, stop=True)
                            return ins
                        T(sc, [bhp[kb]], [pb[0], pb[1]])
                        V(lambda e: e.tensor_copy(out=Ssb[:, 0:256], in_=psum[0][:, 0:256]), [pb[0]], w=[bS])
                        for i, c in enumerate(range(c0, c1)):
                            bank, off = (0, 256 + i * 128) if i < 2 else (1, (i - 2) * 128)
                            for wi in range(2):
                                w = 2 * c + wi
                                d = w - 4 - 2 * tq + 8
                                V(lambda e, bank=bank, off=off, wi=wi, w=w, d=d, i=i, tq=tq, kb=kb, hh=hh: e.scalar_tensor_tensor(
                                    out=Ssb[:, 256 + i * 128 + wi * 64:256 + i * 128 + wi * 64 + 64], in0=psum[bank][:, off + wi * 64:off + wi * 64 + 64],
                                    scalar=rm[:, tq, w:w + 1], in1=tt2[kb][:, hh, d, :], op0=ALU.add, op1=ALU.add), [pb[bank], brm, bhp[kb]], pw=[bS])
                        softmax_pv(256 + nloc, [12, 13] + list(range(c0, c1)), t, h, pl)
            if dbg_stage == ("odd", 1):
                raise StopBuild()
            subphase(mark2)
            aT = [carve([8, 128], BF16), carve([8, 128], BF16)]; baT = [Buf("aT0"), Buf("aT1")]

            def prep(t):
                k = t % 2

                def trs(e):
                    ins = None
                    for kc in range(8):
                        ins = e.transpose(out=psb(5)[:, kc * 128:(kc + 1) * 128], in_=ao[:, t, kc * 128:(kc + 1) * 128], identity=identb[:])
                    return ins
                T(trs, [bao, bconst], [pb[5]])
                A(lambda e: e.copy(out=aT[k], in_=psb(5)[:].rearrange("p (a b) -> p a b", a=8)), [pb[5]], w=[baT[k]])
                return (lambda kc: aT[k][:, kc, :]), [baT[k]]
            post_mixer(2 * l, prep, w_oc[i2], gate1, bgate1)

        def moe_layer(l, v):
            w_gu = lazy("w_gu", [4, 32, 1024, 2048]); w_dn = lazy("w_dn", [4, 32, 1024, 1024]); b_gu = lazy("b_gu", [4, 32, 2048]); b_dn = lazy("b_dn", [4, 32, 1024])
            w_r = lazy("w_r", [4, 128, 8, 32]); b_r = lazy("b_router", [4, 32])
            new_phase()
            mbias[0] = [(carve([512]), Buf("mbb0")), (carve([512]), Buf("mbb1"))]
            shift2 = [carve([1024]), carve([1024])]; scale2 = [carve([1024]), carve([1024])]; gate2 = [carve([1024]), carve([1024])]; bm2 = Buf("mod2")
            mod_rows(l, [3, 4, 5], [shift2, scale2, gate2], bm2)
            wr = carve([8, 32]); br = carve([32]); bwr = Buf("wr")
            DS("wr0", lambda e: e.dma_start(out=wr, in_=w_r[l]), w=[bwr])
            bload("wr1", br, b_r[l:l + 1, :], bwr, pw=True)
            gates = carve([NT, 4]); bgates = Buf("gates"); desti = desti_t; bdesti = Buf("desti")
            maskb = carve([NT, 32], BF16); bmask = Buf("mask")
            mark = apos[0]
            hf = [carve([1024]), carve([1024])]; hb = [hb_t[:, 0, :], hb_t[:, 1, :]]; bh = [Buf("h0"), Buf("h1")]
            hTr = carve([8, 128]); bhTr = Buf("hTr")
            lg = carve([32]); top8 = carve([8]); e4 = carve([8]); Dm = carve([32]); junk = carve([32]); destf = carve([4]); brt = Buf("rt")
            for t in range(NT):
                s_ = SETOF(t); k = t % 2
                V(lambda e, t=t, k=k, s_=s_: e.tensor_tensor(out=hf[k], in0=xres[:, t, :], in1=scale2[s_], op=ALU.mult), [bx[t], bm2], w=[bh[k]])
                V(lambda e, k=k, s_=s_: e.tensor_tensor(out=hf[k], in0=hf[k], in1=shift2[s_], op=ALU.add), [bm2, bh[k]], pw=[bh[k]])
                A(lambda e, k=k: e.copy(out=hb[k], in_=hf[k]), [bh[k]], pw=[bh[k]])
                for half in range(2):
                    bank = 4 + half

                    def trs(e, half=half, bank=bank, k=k):
                        ins = None
                        for q in range(4):
                            kc = half * 4 + q
                            ins = e.transpose(out=psum[bank][:, q * 128:(q + 1) * 128], in_=hf[k][:, kc * 128:(kc + 1) * 128], identity=ident[:])
                        return ins
                    T(trs, [bh[k], bconst], [pb[bank]])
                    V(lambda e, half=half, bank=bank: e.tensor_copy(out=hTr[:, half * 4:(half + 1) * 4, :], in_=psum[bank][:].rearrange("p (a b) -> p a b", a=4)),
                      [pb[bank]], w=[bhTr] if half == 0 else (), pw=[bhTr] if half else ())
                T(mm8(psum[6][:, 0:32], lambda kc: hTr[:, kc, :], lambda kc: wr[:, kc, :]), [bhTr, bwr], [pb[6]])
                V(lambda e: e.tensor_tensor(out=lg, in0=psum[6][:, 0:32], in1=br, op=ALU.add), [pb[6], bwr], w=[brt])
                V(lambda e: e.max(out=top8, in_=lg), [brt], pw=[brt])
                V(lambda e: e.tensor_scalar(out=e4[:, 4:5], in0=top8[:, 0:1], scalar1=-1.0, scalar2=None, op0=ALU.mult), [brt], pw=[brt])
                A(lambda e: e.activation(out=e4[:, 0:4], in_=top8[:, 0:4], func=AF.Exp, bias=e4[:, 4:5], scale=1.0, accum_out=e4[:, 5:6]), [brt], pw=[brt])
                V(lambda e: e.reciprocal(out=e4[:, 6:7], in_=e4[:, 5:6]), [brt], pw=[brt])
                V(lambda e, t=t: e.tensor_scalar(out=gates[:, t, :], in0=e4[:, 0:4], scalar1=e4[:, 6:7], scalar2=None, op0=ALU.mult), [brt], pw=[bgates])
                V(lambda e, t=t: e.tensor_scalar(out=maskb[:, t, :], in0=lg, scalar1=top8[:, 3:4], scalar2=None, op0=ALU.is_ge), [brt], pw=[bmask])

                def posmm(e, t=t):
                    ins = e.matmul(psum[7][:, 0:32], lhsT=tri[:], rhs=maskb[:, t, :], start=True, stop=(t == 0))
                    for j in range(t):
                        ins = e.matmul(psum[7][:, 0:32], lhsT=onesb[:], rhs=maskb[:, j, :], start=False, stop=(j == t - 1))
                    return ins
                T(posmm, [bmask, bconst], [pb[7]])
                V(lambda e: e.scalar_tensor_tensor(out=Dm, in0=psum[7][:, 0:32], scalar=float(CAP - 1), in1=ecap[:], op0=ALU.min, op1=ALU.add), [pb[7], bconst], pw=[brt])
                for kk in range(4):
                    V(lambda e, kk=kk: e.scalar_tensor_tensor(out=junk, in0=lg, scalar=top8[:, kk:kk + 1], in1=Dm, op0=ALU.is_equal, op1=ALU.mult, accum_out=destf[:, kk:kk + 1]),
                      [brt], pw=[brt])
                V(lambda e, t=t: e.tensor_copy(out=desti[:, t, :], in_=destf), [brt], pw=[bdesti])
                for kk in range(4):
                    DP(f"sc{kk}", lambda e, t=t, kk=kk, k=k: e.indirect_dma_start(out=xdisp, out_offset=bass.IndirectOffsetOnAxis(ap=desti[:, t, kk:kk + 1], axis=0),
                                                                                 in_=hb[k], in_offset=None, bounds_check=getreg(e, NSLOT - 1), oob_is_err=False),
                       [bh[k], bdesti], w=[bxdisp] if (t == 0 and kk == 0) else (), pw=() if (t == 0 and kk == 0) else [bxdisp])
            subphase(mark)
            bgt = [carve([2048]), carve([2048])]; bdt = [carve([1024]), carve([1024])]; bbias = [Buf("bias0"), Buf("bias1")]
            xg = [carve([1024], BF16), carve([1024], BF16)]; bxg = [Buf("xg0"), Buf("xg1")]
            xgT = [carve([8, 128], BF16) for _ in range(TPE)]; bxgT = [Buf(f"xgT{j}") for j in range(TPE)]
            act = [carve([1024], BF16) for _ in range(TPE)]; bact = [Buf(f"act{j}") for j in range(TPE)]
            actT = [carve([8, 128], BF16) for _ in range(TPE)]; bactT = [Buf(f"actT{j}") for j in range(TPE)]
            ysb = [carve([1024]), carve([1024])]; bys = [Buf("ys0"), Buf("ys1")]
            tg = [carve([256]), carve([256])]; tl = [carve([256]), carve([256])]; sgm = [carve([256]), carve([256])]; bsw = [Buf("sw0"), Buf("sw1")]
            first_y = True
            for ex in range(NE):
                eb = ex % 2
                bload(f"bg{eb}", bgt[eb], b_gu[l, ex:ex + 1, :], bbias[eb])
                bload(f"bd{eb}", bdt[eb], b_dn[l, ex:ex + 1, :], bbias[eb], pw=True)
                for j in range(TPE):
                    kx = j % 2
                    row0 = ex * CAP + j * 128
                    DS(f"xg{kx}", lambda e, kx=kx, row0=row0: e.dma_start(out=xg[kx], in_=xdisp[row0:row0 + 128, :]), [bxdisp], w=[bxg[kx]])

                    def trs(e, kx=kx):
                        ins = None
                        for kc in range(8):
                            ins = e.transpose(out=psb(4)[:, kc * 128:(kc + 1) * 128], in_=xg[kx][:, kc * 128:(kc + 1) * 128], identity=identb[:])
                        return ins
                    T(trs, [bxg[kx], bconst], [pb[4]])
                    A(lambda e, j=j: e.copy(out=xgT[j], in_=psb(4)[:].rearrange("p (a b) -> p a b", a=8)), [pb[4]], w=[bxgT[j]])
                for c in range(4):
                    wt, wbuf = wload([(rows8(w_gu[l, ex][:, c * 256:(c + 1) * 256]), 0), (rows8(w_gu[l, ex][:, 1024 + c * 256:1024 + (c + 1) * 256]), 256)])
                    for j in range(TPE):
                        k = (c * TPE + j) % 2
                        T(mm8(psum[k][:], lambda kc, j=j: xgT[j][:, kc, :], lambda kc, wt=wt: wt[:, kc, :]), [bxgT[j], wbuf], [pb[k]])
                        V(lambda e, k=k, eb=eb, c=c: e.tensor_tensor(out=tg[k], in0=psum[k][:, 0:256], in1=bgt[eb][:, c * 256:(c + 1) * 256], op=ALU.add), [pb[k], bbias[eb]], w=[bsw[k]])
                        V(lambda e, k=k, eb=eb, c=c: e.tensor_tensor(out=tl[k], in0=psum[k][:, 256:512], in1=bgt[eb][:, 1024 + c * 256:1024 + (c + 1) * 256], op=ALU.add), [pb[k], bbias[eb]], pw=[bsw[k]])
                        V(lambda e, k=k: e.tensor_scalar(out=tg[k], in0=tg[k], scalar1=7.0, scalar2=None, op0=ALU.min), [bsw[k]], pw=[bsw[k]])
                        V(lambda e, k=k: e.tensor_scalar(out=tl[k], in0=tl[k], scalar1=7.0, scalar2=-7.0, op0=ALU.min, op1=ALU.max), [bsw[k]], pw=[bsw[k]])
                        A(lambda e, k=k: e.activation(out=sgm[k], in_=tg[k], func=AF.Sigmoid, scale=1.702), [bsw[k]], pw=[bsw[k]])
                        V(lambda e, k=k: e.scalar_tensor_tensor(out=tl[k], in0=tl[k], scalar=1.0, in1=tg[k], op0=ALU.add, op1=ALU.mult), [bsw[k]], pw=[bsw[k]])
                        V(lambda e, k=k, j=j, c=c: e.tensor_tensor(out=act[j][:, c * 256:(c + 1) * 256], in0=tl[k], in1=sgm[k], op=ALU.mult), [bsw[k]],
                          w=[bact[j]] if c == 0 else (), pw=[bact[j]] if c else ())
                for j in range(TPE):
                    def trs2(e, j=j):
                        ins = None
                        for kc in range(8):
                            ins = e.transpose(out=psb(5)[:, kc * 128:(kc + 1) * 128], in_=act[j][:, kc * 128:(kc + 1) * 128], identity=identb[:])
                        return ins
                    T(trs2, [bact[j], bconst], [pb[5]])
                    A(lambda e, j=j: e.copy(out=actT[j], in_=psb(5)[:].rearrange("p (a b) -> p a b", a=8)), [pb[5]], w=[bactT[j]])
                wd = [wload([(rows8(w_dn[l, ex][:, c * 512:(c + 1) * 512]), 0)]) for c in range(2)]
                for j in range(TPE):
                    ky = j % 2
                    for c in range(2):
                        bank = 2 + c
                        T(mm8(psum[bank][:], lambda kc, j=j: actT[j][:, kc, :], lambda kc, c=c, wd=wd: wd[c][0][:, kc, :]), [bactT[j], wd[c][1]], [pb[bank]])
                        V(lambda e, bank=bank, ky=ky, c=c, eb=eb: e.tensor_tensor(out=ysb[ky][:, c * 512:(c + 1) * 512], in0=psum[bank][:], in1=bdt[eb][:, c * 512:(c + 1) * 512], op=ALU.add),
                          [pb[bank], bbias[eb]], w=[bys[ky]] if c == 0 else (), pw=[bys[ky]] if c else ())
                    row0 = ex * CAP + j * 128
                    DS(f"ys{ky}", lambda e, ky=ky, row0=row0: e.dma_start(out=ybuf[row0:row0 + 128, :], in_=ysb[ky]), [bys[ky]], w=[bybuf] if first_y else (), pw=() if first_y else [bybuf])
                    first_y = False
            subphase(mark)
            lg_ = carve([1024]); lb_ = carve([1024]); blg = Buf("lg"); blb = Buf("lb")
            bload("lng", lg_, ln_g[2 * l + 1:2 * l + 2, :], blg)
            bload("lnb", lb_, ln_b[2 * l + 1:2 * l + 2, :], blb)
            yk = [carve([1024]) for _ in range(4)]; byk = [Buf(f"yk{q}") for q in range(4)]
            acc = carve([1024]); bacc = Buf("acc"); tmpc = carve([16])
            for t in range(NT):
                s_ = SETOF(t)
                for kk in range(4):
                    DP(f"gy{kk}", lambda e, t=t, kk=kk: e.indirect_dma_start(out=yk[kk], out_offset=None, in_=ybuf, in_offset=bass.IndirectOffsetOnAxis(ap=desti[:, t, kk:kk + 1], axis=0),
                                                                            bounds_check=getreg(e, NSLOT - 1), oob_is_err=False), [bybuf, bdesti], w=[byk[kk]])
                V(lambda e, t=t: e.tensor_scalar(out=acc, in0=yk[0], scalar1=gates[:, t, 0:1], scalar2=None, op0=ALU.mult), [byk[0], bgates], w=[bacc])
                for kk in range(1, 4):
                    V(lambda e, t=t, kk=kk: e.scalar_tensor_tensor(out=acc, in0=yk[kk], scalar=gates[:, t, kk:kk + 1], in1=acc, op0=ALU.mult, op1=ALU.add), [byk[kk], bgates, bacc], pw=[bacc])
                V(lambda e, s_=s_: e.tensor_tensor(out=acc, in0=acc, in1=gate2[s_], op=ALU.mult), [bacc, bm2], pw=[bacc])
                V(lambda e, t=t: e.scalar_tensor_tensor(out=acc, in0=xres[:, t, :], scalar=ALPHA, in1=acc, op0=ALU.mult, op1=ALU.add), [bx[t], bacc], pw=[bacc])
                layer_norm(acc, 1024, lg_, lb_, xres[:, t, :], [bacc, blg, blb], bx[t], tmpc)

        stop = False
        for l in range(nlayers):
            i2 = l // 2
            last = (l == nlayers - 1)
            src = xin if l == 0 else XS[l % 2]
            bsrc = [Buf("xin")] * NV if l == 0 else bXS[l % 2]
            dst = xout if last else XS[(l + 1) % 2]
            bdst = None if last else bXS[(l + 1) % 2]
            for v in range(nv):
                new_phase()
                for t in range(NT):
                    DS(f"x{t % 4}", lambda e, t=t, v=v, src=src: e.dma_start(out=xres[:, t, :], in_=src[v][t * 128:(t + 1) * 128, :]), [bsrc[v]], w=[bx[t]])
                mbias[0] = [(carve([512]), Buf("mbb0")), (carve([512]), Buf("mbb1"))]
                bmodc = Buf("modT")
                mod_cols(l, bmodc)
                gate1 = [carve([1024]), carve([1024])]; bgate1 = Buf("gate1")
                mod_rows(l, [2], [gate1], bgate1)
                if l % 2 == 0 and not (dbg_stage is not None and dbg_stage[0] == "odd"):
                    mixer_even(l, i2, v, src, bsrc, bmodc, gate1, bgate1)
                else:
                    try:
                        mixer_odd(l, i2, v, src, bsrc, bmodc, gate1, bgate1)
                    except StopBuild:
                        pass
                if not (dbg_stage == ("mix", l) or (dbg_stage is not None and dbg_stage[0] == "odd")):
                    moe_layer(l, v)
                for t in range(NT):
                    if last:
                        DS(f"o{t % 4}", lambda e, t=t, v=v: e.dma_start(out=xout[v][t * 128:(t + 1) * 128, :], in_=xres[:, t, :]), [bx[t]], out=True)
                    else:
                        DS(f"o{t % 4}", lambda e, t=t, v=v, dst=dst: e.dma_start(out=dst[v][t * 128:(t + 1) * 128, :], in_=xres[:, t, :]), [bx[t]],
                           w=[bdst[v]] if t == 0 else (), pw=() if t == 0 else [bdst[v]])
        S.emit(st)
    return nc, decl


_CACHE = {}


def kernel(**inputs):
    inp = {k: np.asarray(v) for k, v in inputs.items()}
    if "nc" not in _CACHE:
        _CACHE["nc"] = build()
    nc, decl = _CACHE["nc"]
    in_maps = _in_maps(inp, decl)
    res = run_bass_kernel_spmd(nc, in_maps, core_ids=list(range(NCORES)))
    y_prompt = np.zeros((16, 256, D), np.float32)
    y_sample = np.zeros((2, 4096, D), np.float32)
    nk = np.zeros((16, 2, 256, 16, 64), np.float32)
    nv_ = np.zeros((16, 2, 256, 16, 64), np.float32)
    for c in range(NCORES):
        r = res.results[c]
        for v in range(NV):
            xo = r["xout"][v]
            y_prompt[8 * c + 2 * v] = xo[0:256]
            y_prompt[8 * c + 2 * v + 1] = xo[256:512]
            y_sample[c, 1024 * v:1024 * (v + 1)] = xo[512:]
            nk[8 * c + 2 * v:8 * c + 2 * v + 2] = r["nck"][v].reshape(2, 2, 256, 16, 64)
            nv_[8 * c + 2 * v:8 * c + 2 * v + 2] = r["ncv"][v].reshape(2, 2, 256, 16, 64)
    return (y_prompt, y_sample, nk, nv_)
```
